# Optimizing a Trainium2 kernel written in Bass

```python
import jax, jax.numpy as jnp
from jax import lax
import numpy as np

D_MODEL = 4096
BATCH = 1
SEQ = 8192
DEPTH = 2

MLSTM_INNER = 2 * D_MODEL
MLSTM_HEADS = 8
MLSTM_HEAD_DIM = MLSTM_INNER // MLSTM_HEADS
MLSTM_QKV_BLOCK = 4
MLSTM_CONV_WIDTH = 4
MLSTM_CHUNK = 64
SG_CHUNK = 128
SG_GROUPS = 8
SG_WIDTH = 2 * D_MODEL
SG_GROUP_DIM = SG_WIDTH // SG_GROUPS
N_MIXERS = 2
DENSE_FF = 14336
N_EXPERTS = 8
TOP_K = 2
EXPERT_FF = 4096
ALPHA = (2 * DEPTH) ** 0.25
BETA = (8 * DEPTH) ** -0.25
LN_EPS = 1e-5

kernel_name = "hybrid_mlstm_spatialgate_moe_deepnorm"


def layer_norm(x, g, b):
    xf = x.astype(jnp.float32)
    mu = xf.mean(-1, keepdims=True)
    var = jnp.square(xf - mu).mean(-1, keepdims=True)
    return ((xf - mu) * lax.rsqrt(var + LN_EPS) * g + b).astype(x.dtype)


def causal_depthwise_conv(x, w, b):
    out = lax.conv_general_dilated(
        x, w[:, None, :], window_strides=(1,), padding=[(MLSTM_CONV_WIDTH - 1, 0)],
        dimension_numbers=("NWC", "WIO", "NWC"), feature_group_count=x.shape[-1])
    return out + b


def block_diag_proj(x, w):
    B, S, C = x.shape
    xr = x.reshape(B, S, C // MLSTM_QKV_BLOCK, MLSTM_QKV_BLOCK)
    return jnp.einsum("bsnc,ncd->bsnd", xr, w).reshape(B, S, C)


def mlstm_chunkwise(q, k, v, ig, lf):
    B, S, H, dh = q.shape
    L = MLSTM_CHUNK
    nc = S // L

    def to_chunks(t):
        t = t.reshape((B, nc, L, H) + t.shape[3:])
        return jnp.moveaxis(jnp.moveaxis(t, 1, 0), 3, 2)

    causal = jnp.tril(jnp.ones((L, L), dtype=bool))

    def step(carry, inp):
        C, n, m = carry
        qc, kc, vc, igc, lfc = inp
        bcum = jnp.cumsum(lfc, axis=-1)
        dlog = bcum[..., :, None] - bcum[..., None, :] + igc[..., None, :]
        dlog = jnp.where(causal, dlog, -jnp.inf)
        inter_log = bcum + m[..., None]
        m_t = jnp.maximum(inter_log, dlog.max(-1))
        dw = jnp.exp(dlog - m_t[..., None])
        inter_w = jnp.exp(inter_log - m_t)
        scores = jnp.einsum("bhtd,bhsd->bhts", qc, kc) * dw
        num = jnp.einsum("bhts,bhsd->bhtd", scores, vc) \
            + inter_w[..., None] * jnp.einsum("bhtd,bhde->bhte", qc, C)
        den = scores.sum(-1) + inter_w * jnp.einsum("bhtd,bhd->bht", qc, n)
        h = num / jnp.maximum(jnp.abs(den), jnp.exp(-m_t))[..., None]
        b_last = bcum[..., -1]
        wlog = b_last[..., None] - bcum + igc
        m_new = jnp.maximum(b_last + m, wlog.max(-1))
        ws = jnp.exp(wlog - m_new[..., None])
        cw = jnp.exp(b_last + m - m_new)
        kw = kc * ws[..., None]
        C_new = cw[..., None, None] * C + jnp.einsum("bhsd,bhse->bhde", kw, vc)
        n_new = cw[..., None] * n + kw.sum(2)
        return (C_new, n_new, m_new), h

    init = (jnp.zeros((B, H, dh, dh), jnp.float32), jnp.zeros((B, H, dh), jnp.float32),
            jnp.zeros((B, H), jnp.float32))
    _, hs = lax.scan(step, init, (to_chunks(q), to_chunks(k), to_chunks(v), to_chunks(ig), to_chunks(lf)))
    hs = jnp.moveaxis(jnp.moveaxis(hs, 0, 1), 2, 3)
    return hs.reshape(B, S, H, dh)


def mlstm_mixer(x, w_in, conv_w, conv_b, w_q, w_k, w_v, w_gates, b_igate, b_fgate, head_norm_g, skip, w_out):
    B, S, _ = x.shape
    xm, z = jnp.split(x @ w_in, 2, axis=-1)
    xc = jax.nn.silu(causal_depthwise_conv(xm, conv_w, conv_b))
    q = block_diag_proj(xc, w_q)
    k = block_diag_proj(xc, w_k)
    v = block_diag_proj(xm, w_v)
    gates = (q @ w_gates[0] + k @ w_gates[1] + v @ w_gates[2]).astype(jnp.float32)
    ig = gates[..., :MLSTM_HEADS] + b_igate
    lf = jax.nn.log_sigmoid(gates[..., MLSTM_HEADS:] + b_fgate)
    hd = (B, S, MLSTM_HEADS, MLSTM_HEAD_DIM)
    h = mlstm_chunkwise(q.reshape(hd).astype(jnp.float32),
                        k.reshape(hd).astype(jnp.float32) * MLSTM_HEAD_DIM ** -0.5,
                        v.reshape(hd).astype(jnp.float32), ig, lf)
    mu = h.mean(-1, keepdims=True)
    var = jnp.square(h - mu).mean(-1, keepdims=True)
    h = ((h - mu) * lax.rsqrt(var + LN_EPS)).reshape(B, S, MLSTM_INNER) * head_norm_g
    h = jax.nn.sigmoid(z) * (h.astype(x.dtype) + skip * xc)
    return h @ w_out


def spatial_gating_mixer(x, w_in, b_in, norm_g, norm_b, w_s, b_s, w_out):
    B, S, _ = x.shape
    u, v = jnp.split(jax.nn.gelu(x @ w_in + b_in), 2, axis=-1)
    v = layer_norm(v, norm_g, norm_b)
    nc = S // SG_CHUNK
    v = v.reshape(B, nc, SG_CHUNK, SG_GROUPS, SG_GROUP_DIM)
    causal = jnp.tril(jnp.ones((SG_CHUNK, SG_CHUNK), dtype=bool))
    w_causal = jnp.where(causal[None], w_s, 0.0)
    sv = jnp.einsum("gts,bcsgd->bctgd", w_causal, v) + b_s.T[:, :, None]
    return (u * sv.reshape(B, S, SG_WIDTH)) @ w_out


def swiglu(x, w1, w3, w2):
    return (jax.nn.silu(x @ w1) * (x @ w3)) @ w2


def moe_swiglu(x, router_w, router_b, w1, w3, w2):
    B, S, D = x.shape
    xt = x.reshape(B * S, D)
    logits = (xt @ router_w).astype(jnp.float32) + router_b
    top_logits, top_idx = lax.top_k(logits, TOP_K)
    top_w = jax.nn.softmax(top_logits, axis=-1)
    gates = jnp.sum(jax.nn.one_hot(top_idx, N_EXPERTS, dtype=jnp.float32) * top_w[..., None], axis=1)
    y = jnp.zeros_like(xt)
    for e in range(N_EXPERTS):
        y = y + gates[:, e:e + 1].astype(x.dtype) * swiglu(xt, w1[e], w3[e], w2[e])
    return y.reshape(B, S, D)


def setup_inputs(seed: int = 0) -> dict:
    key = jax.random.key(seed)
    ks = iter(jax.random.split(key, 48))

    def nrm(shape, scale):
        return jax.random.normal(next(ks), shape, jnp.float32) * scale

    def gain(n):
        return 1.0 + nrm((n,), 0.02)

    nb = MLSTM_INNER // MLSTM_QKV_BLOCK
    return {
        "x": nrm((BATCH, SEQ, D_MODEL), 1.0),
        "l0_mix_w_in": nrm((D_MODEL, 2 * MLSTM_INNER), D_MODEL ** -0.5),
        "l0_conv_w": nrm((MLSTM_CONV_WIDTH, MLSTM_INNER), MLSTM_CONV_WIDTH ** -0.5),
        "l0_conv_b": nrm((MLSTM_INNER,), 0.01),
        "l0_w_q": nrm((nb, MLSTM_QKV_BLOCK, MLSTM_QKV_BLOCK), MLSTM_QKV_BLOCK ** -0.5),
        "l0_w_k": nrm((nb, MLSTM_QKV_BLOCK, MLSTM_QKV_BLOCK), MLSTM_QKV_BLOCK ** -0.5),
        "l0_w_v": nrm((nb, MLSTM_QKV_BLOCK, MLSTM_QKV_BLOCK), MLSTM_QKV_BLOCK ** -0.5),
        "l0_w_gates": nrm((3, MLSTM_INNER, 2 * MLSTM_HEADS), (3 * MLSTM_INNER) ** -0.5),
        "l0_b_igate": nrm((MLSTM_HEADS,), 0.1),
        "l0_b_fgate": jnp.linspace(3.0, 6.0, MLSTM_HEADS, dtype=jnp.float32) + nrm((MLSTM_HEADS,), 0.1),
        "l0_head_norm_g": gain(MLSTM_INNER),
        "l0_skip": gain(MLSTM_INNER),
        "l0_mix_w_out": nrm((MLSTM_INNER, D_MODEL), MLSTM_INNER ** -0.5 * BETA),
        "l0_ln1_g": gain(D_MODEL),
        "l0_ln1_b": nrm((D_MODEL,), 0.02),
        "l0_ffn_w1": nrm((D_MODEL, DENSE_FF), D_MODEL ** -0.5),
        "l0_ffn_w3": nrm((D_MODEL, DENSE_FF), D_MODEL ** -0.5),
        "l0_ffn_w2": nrm((DENSE_FF, D_MODEL), DENSE_FF ** -0.5 * BETA),
        "l0_ln2_g": gain(D_MODEL),
        "l0_ln2_b": nrm((D_MODEL,), 0.02),
        "l1_mix_w_in": nrm((D_MODEL, 2 * SG_WIDTH), D_MODEL ** -0.5),
        "l1_mix_b_in": nrm((2 * SG_WIDTH,), 0.02),
        "l1_sg_norm_g": gain(SG_WIDTH),
        "l1_sg_norm_b": nrm((SG_WIDTH,), 0.02),
        "l1_sg_w": nrm((SG_GROUPS, SG_CHUNK, SG_CHUNK), SG_CHUNK ** -0.5),
        "l1_sg_b": 1.0 + nrm((SG_GROUPS, SG_CHUNK), 0.02),
        "l1_mix_w_out": nrm((SG_WIDTH, D_MODEL), SG_WIDTH ** -0.5 * BETA),
        "l1_ln1_g": gain(D_MODEL),
        "l1_ln1_b": nrm((D_MODEL,), 0.02),
        "l1_router_w": nrm((D_MODEL, N_EXPERTS), D_MODEL ** -0.5),
        "l1_router_b": nrm((N_EXPERTS,), 0.01),
        "l1_exp_w1": nrm((N_EXPERTS, D_MODEL, EXPERT_FF), D_MODEL ** -0.5),
        "l1_exp_w3": nrm((N_EXPERTS, D_MODEL, EXPERT_FF), D_MODEL ** -0.5),
        "l1_exp_w2": nrm((N_EXPERTS, EXPERT_FF, D_MODEL), EXPERT_FF ** -0.5 * BETA),
        "l1_ln2_g": gain(D_MODEL),
        "l1_ln2_b": nrm((D_MODEL,), 0.02),
    }


def reference(x,
              l0_mix_w_in, l0_conv_w, l0_conv_b, l0_w_q, l0_w_k, l0_w_v, l0_w_gates, l0_b_igate, l0_b_fgate,
              l0_head_norm_g, l0_skip, l0_mix_w_out, l0_ln1_g, l0_ln1_b,
              l0_ffn_w1, l0_ffn_w3, l0_ffn_w2, l0_ln2_g, l0_ln2_b,
              l1_mix_w_in, l1_mix_b_in, l1_sg_norm_g, l1_sg_norm_b, l1_sg_w, l1_sg_b, l1_mix_w_out,
              l1_ln1_g, l1_ln1_b,
              l1_router_w, l1_router_b, l1_exp_w1, l1_exp_w3, l1_exp_w2, l1_ln2_g, l1_ln2_b):
    token_mixers = (
        lambda h: mlstm_mixer(h, l0_mix_w_in, l0_conv_w, l0_conv_b, l0_w_q, l0_w_k, l0_w_v, l0_w_gates,
                              l0_b_igate, l0_b_fgate, l0_head_norm_g, l0_skip, l0_mix_w_out),
        lambda h: spatial_gating_mixer(h, l1_mix_w_in, l1_mix_b_in, l1_sg_norm_g, l1_sg_norm_b,
                                       l1_sg_w, l1_sg_b, l1_mix_w_out),
    )
    channel_mixers = (
        lambda h: swiglu(h, l0_ffn_w1, l0_ffn_w3, l0_ffn_w2),
        lambda h: moe_swiglu(h, l1_router_w, l1_router_b, l1_exp_w1, l1_exp_w3, l1_exp_w2),
    )
    post_norms = ((l0_ln1_g, l0_ln1_b, l0_ln2_g, l0_ln2_b), (l1_ln1_g, l1_ln1_b, l1_ln2_g, l1_ln2_b))
    for layer in range(DEPTH):
        g1, b1, g2, b2 = post_norms[layer]
        x = layer_norm(ALPHA * x + token_mixers[layer % N_MIXERS](x), g1, b1)
        x = layer_norm(ALPHA * x + channel_mixers[layer](x), g2, b2)
    return x
```

```python
import numpy as np
from contextlib import ExitStack
import concourse.bass as bass
import concourse.mybir as mybir
from concourse.bass_utils import run_bass_kernel_spmd
import ml_dtypes

F32 = mybir.dt.float32
BF16 = mybir.dt.bfloat16
AF = mybir.ActivationFunctionType
ALU = mybir.AluOpType
AX = mybir.AxisListType
NPBF = ml_dtypes.bfloat16
LN_EPS = 1e-5
DEPTH = 2
ALPHA = (2 * DEPTH) ** 0.25


class Prog:
    def __init__(self):
        self.ops = []

    def add(self, eng, fn, reads=(), writes=(), dma=False):
        self.ops.append(dict(eng=eng, fn=fn, reads=tuple(reads), writes=tuple(writes), dma=dma))

    def build(self, nc, stack, K=16):
        ops = self.ops
        n = len(ops)
        last_w = {}
        readers = {}
        deps = [None] * n
        for i, o in enumerate(ops):
            d = set()
            for b in o['reads']:
                if b in last_w:
                    d.add(last_w[b])
            for b in o['writes']:
                if b in last_w:
                    d.add(last_w[b])
                for r in readers.get(b, ()):
                    d.add(r)
            for b in o['reads']:
                readers.setdefault(b, []).append(i)
            for b in o['writes']:
                last_w[b] = i
                readers[b] = []
            d.discard(i)
            if o['eng'] == 'pe' and not o['dma']:
                d = {j for j in d if not (ops[j]['eng'] == 'pe' and not ops[j]['dma'])}
            deps[i] = d
        needed = set()
        for d in deps:
            needed |= d
        for i, o in enumerate(ops):
            if o['dma']:
                needed.add(i)
        engs = ['pe', 'act', 'dve', 'pool', 'sp']
        esem = {e: stack.enter_context(nc.semaphore("se_" + e)) for e in engs}
        dsem = {e: [stack.enter_context(nc.semaphore("sd_%s%d" % (e, k))) for k in range(K)] for e in ('pool', 'sp', 'act')}
        ecnt = {e: 0 for e in engs}
        dcnt = {e: 0 for e in dsem}
        sig = [None] * n
        for i, o in enumerate(ops):
            if i not in needed:
                continue
            if o['dma']:
                q = o['eng']
                k = dcnt[q]
                dcnt[q] += 1
                sig[i] = (dsem[q][k % K], 16 * (k // K + 1), 16)
            else:
                ecnt[o['eng']] += 1
                sig[i] = (esem[o['eng']], ecnt[o['eng']], 1)
        self.stats = dict(n=n, ecnt=dict(ecnt), dcnt=dict(dcnt))

        def emit(engname, e):
            waited = {}
            for i, o in enumerate(ops):
                if o['eng'] != engname:
                    continue
                for j in sorted(deps[i]):
                    s, v, _ = sig[j]
                    key = id(s)
                    if waited.get(key, 0) < v:
                        e.wait_ge(s, v)
                        waited[key] = v
                ins = o['fn'](e) if o['fn'] is not None else None
                if sig[i] is not None:
                    s, v, inc = sig[i]
                    assert ins is not None
                    ins.then_inc(s, inc)

        with nc.Block() as block:
            @block.tensor
            def _(e):
                emit('pe', e)

            @block.scalar
            def _(e):
                emit('act', e)

            @block.vector
            def _(e):
                emit('dve', e)

            @block.gpsimd
            def _(e):
                emit('pool', e)

            @block.sync
            def _(e):
                emit('sp', e)


class Ring:
    def __init__(self, tiles, name):
        self.tiles = tiles
        self.name = name
        self.i = 0

    def next(self):
        k = self.i % len(self.tiles)
        self.i += 1
        return self.tiles[k], (self.name, k)


class Ctx:
    pass


def mk_common(nc, stack, P, npsum=6):
    c = Ctx()
    c.nc = nc
    c.P = P
    c.stack = stack
    c.psf = Ring([stack.enter_context(nc.psum_tensor("psf%d" % i, [128, 512], F32)) for i in range(npsum)], "psf")
    c.psb = Ring([stack.enter_context(nc.psum_tensor("psb%d" % i, [128, 1024], BF16)) for i in range(8 - npsum)], "psb")
    return c


def sb(c, name, shape, dt):
    return c.stack.enter_context(c.nc.sbuf_tensor(name + '_sb', shape, dt))


def bcast_rows(ap2d, nparts):
    return ap2d.to_broadcast([nparts, ap2d.shape[1]])


def build_l12(cfg, mode):
    D, S, T, KH = cfg['D'], cfg['S'], cfg['T'], cfg['KH']
    H = 8
    INNER = 2 * D
    dh = INNER // H
    KC = dh // 128
    KD = D // 128
    NT = T // 128
    NTILES = S // T
    FB = KH * 128
    NKP = KD // KH
    EW = min(512, dh)
    NEH = dh // EW
    SC = float(dh) ** -0.5
    NCG = (2 * dh) // 512
    assert (2 * dh) % 512 == 0 and KD % KH == 0
    nc = bass.Bass("TRN2", target_bir_lowering=False)
    dr = lambda name, shape, dt=F32: nc.dram_tensor(name, shape, dt, kind="ExternalInput").ap()
    xT_d = dr("xT", [D, S])
    win_d = dr("w_in_h", [D, 2 * dh])
    convw_d = dr("convw", [128, KC, 4]); convb_d = dr("convb", [128, KC])
    wq_d = dr("wq_bd", [128, KC, 128]); wk_d = dr("wk_bd", [128, KC, 128]); wv_d = dr("wv_bd", [128, KC, 128])
    identb_d = dr("identb", [128, 128], BF16); identf_d = dr("identf", [128, 128])
    if mode == 1:
        wg_d = dr("wg", [128, 3, KC, 16])
        pg_d = nc.dram_tensor("pg", [16, S], F32, kind="ExternalOutput").ap()
    else:
        pgs_d = dr("pgs", [S, 16])
        gb_d = dr("gbias", [128, 2])
        gn_d = dr("gn", [1, dh]); skip_d = dr("skipcol", [128, KC])
        triu_d = dr("triu", [128, 128])
        negl_d = dr("negmask_ts", [128, 128])
        negu_d = dr("negmask_st", [128, 128])
        hg_d = nc.dram_tensor("hgT", [dh, S], BF16, kind="ExternalOutput").ap()

    P = Prog()
    with ExitStack() as stack:
        c = mk_common(nc, stack, P)
        xT = sb(c, "xT", [128, KD, T], BF16)
        wring = Ring([sb(c, "wp%d" % i, [128, KH, 512], BF16) for i in range(cfg.get('NW', 2))], "wp")
        xmT = sb(c, "xmT", [128, KC, T + 3], F32)
        xmb = sb(c, "xmb", [128, KC, T], BF16)
        sz = sb(c, "sz", [128, KC, T], F32)
        cacc = sb(c, "cacc", [128, T], F32)
        xc = sb(c, "xc", [128, KC, T], F32)
        xcb = sb(c, "xcb", [128, KC, T], BF16)
        qT = sb(c, "qT", [128, KC, T], BF16); kT = sb(c, "kT", [128, KC, T], BF16)
        convw = sb(c, "convw_s", [128, KC, 4], F32); convb = sb(c, "convb_s", [128, KC], F32)
        wst = sb(c, "wst", [128, KC, 128], F32)
        wq = sb(c, "wq_s", [128, KC, 128], BF16); wk = sb(c, "wk_s", [128, KC, 128], BF16); wv = sb(c, "wv_s", [128, KC, 128], BF16)
        identb = sb(c, "identb_s", [128, 128], BF16); identf = sb(c, "identf_s", [128, 128], F32)
        ones_f = sb(c, "ones_f", [128, 128], F32); ones_b = sb(c, "ones_b", [128, 8], BF16)
        epsc = sb(c, "epsc", [128, 1], F32)

        def dma(q, out, in_, reads, writes):
            P.add(q, lambda e, o=out, i=in_: e.dma_start(out=o, in_=i), reads=reads, writes=writes, dma=True)

        def load_wpiece(W, r0, c0):
            t, key = wring.next()
            src = W[r0:r0 + KH * 128, c0:c0 + 512].rearrange("(k p) c -> p k c", p=128)
            dma('pool', t[:], src, reads=(), writes=(key,))
            return t, key

        def mm_group(outap, pairs, reads, writes, start=True, stop=True):
            def fn(e, outap=outap, pairs=pairs, start=start, stop=stop):
                ins = None
                for idx, (l, r) in enumerate(pairs):
                    ins = e.matmul(outap, l, r, start=(start and idx == 0), stop=(stop and idx == len(pairs) - 1))
                return ins
            P.add('pe', fn, reads=reads, writes=writes)

        V = lambda fn, r, w: P.add('dve', fn, r, w)
        A = lambda fn, r, w: P.add('act', fn, r, w)

        dma('sp', identb[:], identb_d, (), ('identb',))
        dma('sp', identf[:], identf_d, (), ('identf',))
        V(lambda e: e.memset(ones_f[:, :], 1.0), (), ('ones_f',))
        V(lambda e: e.memset(ones_b[:, :], 1.0), (), ('ones_b',))
        V(lambda e: e.memset(epsc[:, :], LN_EPS), (), ('epsc',))
        V(lambda e: e.memset(xmT[:, :, 0:3], 0.0), (), ('xmT',))
        dma('sp', convw[:], convw_d, (), ('convw',)); dma('sp', convb[:], convb_d, (), ('convb',))
        for (wd, ws_, nm) in ((wq_d, wq, 'wq'), (wk_d, wk, 'wk'), (wv_d, wv, 'wv')):
            dma('sp', wst[:], wd, (), ('wst',))
            A(lambda e, ws_=ws_: e.activation(out=ws_[:], in_=wst[:], func=AF.Copy), ('wst',), (nm,))
        if mode == 1:
            wgf = sb(c, "wgf", [128, 3, KC, 16], F32); wgb = sb(c, "wgb", [128, 3, KC, 16], BF16)
            vT = sb(c, "vT", [128, KC, T], BF16)
            pgt = sb(c, "pgt", [16, T], F32)
            dma('sp', wgf[:], wg_d, (), ('wgf',))
            A(lambda e: e.activation(out=wgb[:], in_=wgf[:], func=AF.Copy), ('wgf',), ('wgb',))
        else:
            k_tm = sb(c, "k_tm", [128, NT, dh], BF16); v_tm = sb(c, "v_tm", [128, NT, dh], BF16)
            Cf = sb(c, "Cf", [128, KC, dh], F32); Cb = sb(c, "Cb", [128, KC, dh], BF16)
            nf = sb(c, "nf", [128, KC], F32); nb = sb(c, "nb", [128, KC], BF16)
            m_bc = sb(c, "m_bc", [128, 1], F32)
            gbias = sb(c, "gbias", [128, 2], F32)
            gn_bc = sb(c, "gn_bc", [128, dh], F32); skipc = sb(c, "skipc", [128, KC], F32)
            triu = sb(c, "triu", [128, 128], F32); negl = sb(c, "negl", [128, 128], F32); negu = sb(c, "negu", [128, 128], F32)
            pgl = sb(c, "pgl", [128, 8, 2], F32); gs = sb(c, "gs", [128, 2], F32)
            igc = sb(c, "igc", [128, 1], F32); lfc = sb(c, "lfc", [128, 1], F32)
            bcol = sb(c, "bcol", [128, 1], F32); gcol = sb(c, "gcol", [128, 1], F32)
            acol = sb(c, "acol", [128, 1], F32); abcol = sb(c, "abcol", [128, 1], F32)
            dg = sb(c, "dg", [128, 128], F32)
            Gm = sb(c, "Gm", [128, 128], F32)
            a_bc = sb(c, "a_bc", [128, 128], F32)
            mnew = sb(c, "mnew", [128, 1], F32); nalast = sb(c, "nalast", [128, 1], F32)
            dw = sb(c, "dw", [128, 128], F32); iw = sb(c, "iw", [128, 128], F32)
            flo = sb(c, "flo", [128, 1], F32)
            ST = sb(c, "ST", [128, 128], BF16)
            qp = sb(c, "qp", [128, KC, 128], BF16)
            den = sb(c, "den", [128, 1], F32)
            hh = sb(c, "hh", [128, dh], F32); hnb = sb(c, "hnb", [128, dh], BF16)
            hst = sb(c, "hst", [128, NEH, 6], F32)
            mv = sb(c, "mv", [128, 2], F32); rstd = sb(c, "rstd", [128, 1], F32)
            gtmp = sb(c, "gtmp", [128, 128], F32)
            hgt = sb(c, "hgt", [128, KC, T], BF16)
            wsc = sb(c, "wsc", [128, 1], F32); cw = sb(c, "cw", [128, 1], F32)
            kw = sb(c, "kw", [128, dh], BF16)
            dma('sp', gbias[:], gb_d, (), ('gbias',))
            dma('sp', gn_bc[:], bcast_rows(gn_d, 128), (), ('gn_bc',))
            dma('sp', skipc[:], skip_d, (), ('skipc',))
            dma('sp', triu[:], triu_d, (), ('triu',)); dma('sp', negl[:], negl_d, (), ('negl',)); dma('sp', negu[:], negu_d, (), ('negu',))
            V(lambda e: e.memset(Cf[:], 0.0), (), [('Cf', dk) for dk in range(KC)]); V(lambda e: e.memset(Cb[:], 0.0), (), ('Cb',))
            V(lambda e: e.memset(nf[:], 0.0), (), ('nf',)); V(lambda e: e.memset(nb[:], 0.0), (), ('nb',))
            V(lambda e: e.memset(m_bc[:], 0.0), (), ('m_bc',))

        def bc_row(col_ap, colkey, out_sb, outkey):
            V(lambda e: e.tensor_scalar(out=dg[:, :], in0=identf[:, :], scalar1=col_ap, scalar2=None, op0=ALU.mult), ('identf', colkey), ('dg',))
            bank, bkey = c.psf.next()
            mm_group(bank[:, 0:128], [(ones_f[:, :], dg[:, :])], ('ones_f', 'dg'), (bkey,))
            return bank, bkey

        for ti in range(NTILES):
            t0 = ti * T
            dma('pool', xT[:], xT_d[:, t0:t0 + T].rearrange("(k p) t -> p k t", p=128), (), ('xT',))
            for cg in range(NCG):
                banks = [c.psf.next() for _ in range(4)]
                for kp in range(NKP):
                    wt, wkey = load_wpiece(win_d, kp * FB, cg * 512)
                    for ci in range(4):
                        pairs = [(wt[:, k, ci * 128:(ci + 1) * 128], xT[:, kp * KH + k, 0:T]) for k in range(KH)]
                        mm_group(banks[ci][0][:, 0:T], pairs, (wkey, 'xT'), (banks[ci][1],), start=(kp == 0), stop=(kp == NKP - 1))
                for ci in range(4):
                    j = cg * 4 + ci
                    bank, bkey = banks[ci]
                    if j < KC:
                        A(lambda e, bank=bank, j=j: e.activation(out=xmT[:, j, 3:3 + T], in_=bank[:, 0:T], func=AF.Copy), (bkey,), (('xmT', j),))
                    else:
                        A(lambda e, bank=bank, j=j: e.activation(out=sz[:, j - KC, :], in_=bank[:, 0:T], func=AF.Sigmoid), (bkey,), ('sz',))
            for k in range(KC):
                xk = ('xmT', k)
                rd = (xk, 'xmT', 'convw')
                V(lambda e, k=k: e.tensor_scalar(out=cacc[:, :], in0=xmT[:, k, 0:T], scalar1=convw[:, k, 0:1], scalar2=None, op0=ALU.mult), rd, ('cacc',))
                for j in (1, 2, 3):
                    V(lambda e, k=k, j=j: e.scalar_tensor_tensor(out=cacc[:, :], in0=xmT[:, k, j:j + T], scalar=convw[:, k, j:j + 1], in1=cacc[:, :],
                                                                 op0=ALU.mult, op1=ALU.add), rd + ('cacc',), ('cacc',))
                A(lambda e, k=k: e.activation(out=xc[:, k, :], in_=cacc[:, :], func=AF.Silu, bias=convb[:, k:k + 1], scale=1.0), ('cacc', 'convb'), (('xc', k),))
                A(lambda e, k=k: e.activation(out=xcb[:, k, :], in_=xc[:, k, :], func=AF.Copy), (('xc', k),), (('xcb', k),))
                A(lambda e, k=k: e.activation(out=xmb[:, k, :], in_=xmT[:, k, 3:3 + T], func=AF.Copy), (xk,), (('xmb', k),))
                V(lambda e, k=k: e.tensor_copy(out=xmT[:, k, 0:3], in_=xmT[:, k, T:T + 3]), (xk, 'xmT', 'cacc'), (xk,))
            for k in range(KC):
                for (wsb, wn, dst, dn) in ((wq, 'wq', qT, 'qT'), (wk, 'wk', kT, 'kT')):
                    bank, bkey = c.psf.next()
                    mm_group(bank[:, 0:T], [(wsb[:, k, :], xcb[:, k, :])], (wn, ('xcb', k)), (bkey,))
                    A(lambda e, bank=bank, dst=dst, k=k: e.activation(out=dst[:, k, :], in_=bank[:, 0:T], func=AF.Copy), (bkey,), ((dn, k),))
            QK = [('qT', k) for k in range(KC)]; KK = [('kT', k) for k in range(KC)]
            if mode == 1:
                for k in range(KC):
                    bank, bkey = c.psf.next()
                    mm_group(bank[:, 0:T], [(wv[:, k, :], xmb[:, k, :])], ('wv', ('xmb', k)), (bkey,))
                    A(lambda e, bank=bank, k=k: e.activation(out=vT[:, k, :], in_=bank[:, 0:T], func=AF.Copy), (bkey,), (('vT', k),))
                bank, bkey = c.psf.next()
                pairs = []
                for i, src in enumerate((qT, kT, vT)):
                    for k in range(KC):
                        pairs.append((wgb[:, i, k, :], src[:, k, :]))
                mm_group(bank[0:16, 0:T], pairs, ['wgb'] + QK + KK + [('vT', k) for k in range(KC)], (bkey,))
                A(lambda e, bank=bank: e.activation(out=pgt[:, :], in_=bank[0:16, 0:T], func=AF.Copy), (bkey,), ('pgt',))
                dma('sp', pg_d[:, t0:t0 + T], pgt[:, :], ('pgt',), (('pg', ti),))
                continue
            for n in range(NT):
                for (wsb, wn, srcb, sn, dst, dn) in ((wk, 'wk', xcb, 'xcb', k_tm, 'k_tm'), (wv, 'wv', xmb, 'xmb', v_tm, 'v_tm')):
                    for k0 in range(0, KC, 4):
                        kk = min(4, KC - k0)
                        bank, bkey = c.psf.next()
                        for k in range(k0, k0 + kk):
                            mm_group(bank[:, (k - k0) * 128:(k - k0 + 1) * 128], [(srcb[:, k, n * 128:(n + 1) * 128], wsb[:, k, :])],
                                     (wn, (sn, k)), (bkey,))
                        A(lambda e, bank=bank, dst=dst, n=n, k0=k0, kk=kk: e.activation(out=dst[:, n, k0 * 128:(k0 + kk) * 128], in_=bank[:, 0:kk * 128], func=AF.Copy),
                          (bkey,), ((dn, n),))
            for n in range(NT):
                tc0 = t0 + n * 128
                cs = slice(n * 128, (n + 1) * 128)
                dma('sp', pgl[:], pgs_d[tc0:tc0 + 128, :].rearrange("t (c j) -> t c j", j=2), (), ('pgl',))
                V(lambda e: e.tensor_reduce(out=gs[:, :], in_=pgl[:].rearrange("p c j -> p j c"), axis=AX.X, op=ALU.add), ('pgl',), ('gs',))
                V(lambda e: e.tensor_tensor(out=gs[:, :], in0=gs[:, :], in1=gbias[:, :], op=ALU.add), ('gs', 'gbias'), ('gs',))
                V(lambda e: e.tensor_copy(out=igc[:, :], in_=gs[:, 0:1]), ('gs',), ('igc',))
                A(lambda e: e.activation(out=lfc[:, :], in_=gs[:, 1:2], func=AF.Exp, scale=-1.0), ('gs',), ('lfc',))
                V(lambda e: e.tensor_scalar(out=lfc[:, :], in0=lfc[:, :], scalar1=1.0, scalar2=None, op0=ALU.add), ('lfc',), ('lfc',))
                A(lambda e: e.activation(out=lfc[:, :], in_=lfc[:, :], func=AF.Ln), ('lfc',), ('lfc',))
                V(lambda e: e.tensor_scalar(out=lfc[:, :], in0=lfc[:, :], scalar1=-1.0, scalar2=None, op0=ALU.mult), ('lfc',), ('lfc',))
                bank, bkey = c.psf.next()
                mm_group(bank[:, 0:1], [(triu[:, :], lfc[:, :])], ('triu', 'lfc'), (bkey,))
                V(lambda e, bank=bank: e.tensor_copy(out=bcol[:, :], in_=bank[:, 0:1]), (bkey,), ('bcol',))
                V(lambda e: e.tensor_tensor(out=gcol[:, :], in0=igc[:, :], in1=bcol[:, :], op=ALU.subtract), ('igc', 'bcol'), ('gcol',))
                bank, bkey = bc_row(gcol[:, 0:1], 'gcol', None, None)
                V(lambda e, bank=bank: e.tensor_tensor(out=Gm[:, :], in0=bank[:, 0:128], in1=negl[:, :], op=ALU.add), (bkey, 'negl'), ('Gm',))
                V(lambda e: e.tensor_reduce(out=acol[:, :], in_=Gm[:, :], axis=AX.X, op=ALU.max), ('Gm',), ('acol',))
                V(lambda e: e.tensor_tensor(out=acol[:, :], in0=acol[:, :], in1=m_bc[:, :], op=ALU.max), ('acol', 'm_bc'), ('acol',))
                V(lambda e: e.tensor_tensor(out=abcol[:, :], in0=acol[:, :], in1=bcol[:, :], op=ALU.add), ('acol', 'bcol'), ('abcol',))
                bank, bkey = bc_row(acol[:, 0:1], 'acol', None, None)
                A(lambda e, bank=bank: e.activation(out=a_bc[:, :], in_=bank[:, 0:128], func=AF.Copy), (bkey,), ('a_bc',))
                bank, bkey = bc_row(abcol[:, 0:1], 'abcol', None, None)
                V(lambda e, bank=bank: e.tensor_copy(out=mnew[:, :], in_=bank[:, 127:128]), (bkey,), ('mnew',))
                V(lambda e: e.tensor_scalar(out=nalast[:, :], in0=a_bc[:, 127:128], scalar1=-1.0, scalar2=None, op0=ALU.mult), ('a_bc',), ('nalast',))
                V(lambda e: e.tensor_scalar(out=dw[:, :], in0=a_bc[:, :], scalar1=-1.0, scalar2=gcol[:, 0:1], op0=ALU.mult, op1=ALU.add), ('a_bc', 'gcol'), ('dw',))
                V(lambda e: e.tensor_tensor(out=dw[:, :], in0=dw[:, :], in1=negu[:, :], op=ALU.add), ('dw', 'negu'), ('dw',))
                A(lambda e: e.activation(out=dw[:, :], in_=dw[:, :], func=AF.Exp), ('dw',), ('dw',))
                A(lambda e: e.activation(out=iw[:, :], in_=a_bc[:, :], func=AF.Exp, scale=-1.0, bias=m_bc[:, 0:1]), ('a_bc', 'm_bc'), ('iw',))
                A(lambda e: e.activation(out=flo[:, :], in_=abcol[:, :], func=AF.Exp, scale=-1.0), ('abcol',), ('flo',))
                bank, bkey = c.psf.next()
                mm_group(bank[:, 0:128], [(kT[:, k, cs], qT[:, k, cs]) for k in range(KC)], QK + KK, (bkey,))
                V(lambda e, bank=bank: e.scalar_tensor_tensor(out=ST[:, :], in0=bank[:, 0:128], scalar=SC, in1=dw[:, :], op0=ALU.mult, op1=ALU.mult), (bkey, 'dw'), ('ST',))
                for k in range(KC):
                    V(lambda e, k=k, cs=cs: e.tensor_tensor(out=qp[:, k, :], in0=qT[:, k, cs], in1=iw[:, :], op=ALU.mult), (('qT', k), 'iw'), ('qp',))
                nbanks = []
                for eh in range(NEH):
                    bank, bkey = c.psf.next()
                    es = slice(eh * EW, (eh + 1) * EW)
                    pairs = [(ST[:, :], v_tm[:, n, es])] + [(qp[:, k, :], Cb[:, k, es]) for k in range(KC)]
                    mm_group(bank[:, 0:EW], pairs, ('ST', ('v_tm', n), 'qp', 'Cb'), (bkey,))
                    nbanks.append((bank, bkey))
                dbank, dkey = c.psf.next()
                pairs = [(ST[:, :], ones_b[:, 0:1])] + [(qp[:, k, :], nb[:, k:k + 1]) for k in range(KC)]
                mm_group(dbank[:, 0:1], pairs, ('ST', 'ones_b', 'qp', 'nb'), (dkey,))
                A(lambda e, dbank=dbank: e.activation(out=den[:, :], in_=dbank[:, 0:1], func=AF.Abs), (dkey,), ('den',))
                V(lambda e: e.tensor_tensor(out=den[:, :], in0=den[:, :], in1=flo[:, :], op=ALU.max), ('den', 'flo'), ('den',))
                V(lambda e: e.reciprocal(out=den[:, :], in_=den[:, :]), ('den',), ('den',))
                for eh in range(NEH):
                    bank, bkey = nbanks[eh]
                    es = slice(eh * EW, (eh + 1) * EW)
                    A(lambda e, bank=bank, es=es: e.activation(out=hh[:, es], in_=bank[:, 0:EW], func=AF.Identity, scale=den[:, 0:1]), (bkey, 'den'), ('hh',))
                for eh in range(NEH):
                    es = slice(eh * EW, (eh + 1) * EW)
                    V(lambda e, eh=eh, es=es: e.bn_stats(out=hst[:, eh, :], in_=hh[:, es]), ('hh',), ('hst',))
                V(lambda e: e.bn_aggr(out=mv[:, :], in_=hst[:, 0:NEH, :].rearrange("p a b -> p (a b)")), ('hst',), ('mv',))
                A(lambda e: e.activation(out=rstd[:, :], in_=mv[:, 1:2], func=AF.Sqrt, bias=epsc[:, 0:1], scale=1.0), ('mv', 'epsc'), ('rstd',))
                V(lambda e: e.reciprocal(out=rstd[:, :], in_=rstd[:, :]), ('rstd',), ('rstd',))
                V(lambda e: e.tensor_scalar(out=hh[:, :], in0=hh[:, :], scalar1=mv[:, 0:1], scalar2=rstd[:, 0:1], op0=ALU.subtract, op1=ALU.mult), ('hh', 'mv', 'rstd'), ('hh',))
                V(lambda e: e.tensor_tensor(out=hnb[:, :], in0=hh[:, :], in1=gn_bc[:, :], op=ALU.mult), ('hh', 'gn_bc'), ('hnb',))
                for k0 in range(0, KC, 8):
                    kk = min(8, KC - k0)
                    pb, pkey = c.psb.next()

                    def tfn(e, pb=pb, k0=k0, kk=kk):
                        ins = None
                        for k in range(kk):
                            ins = e.transpose(out=pb[:, k * 128:(k + 1) * 128], in_=hnb[:, (k0 + k) * 128:(k0 + k + 1) * 128], identity=identb[:, :])
                        return ins
                    P.add('pe', tfn, ('hnb', 'identb'), (pkey,))
                    for k in range(k0, k0 + kk):
                        V(lambda e, pb=pb, k=k, k0=k0, cs=cs: e.scalar_tensor_tensor(out=gtmp[:, :], in0=xc[:, k, cs], scalar=skipc[:, k:k + 1],
                                                                                    in1=pb[:, (k - k0) * 128:(k - k0 + 1) * 128], op0=ALU.mult, op1=ALU.add),
                          (('xc', k), 'skipc', pkey), ('gtmp',))
                        V(lambda e, k=k, cs=cs: e.tensor_tensor(out=hgt[:, k, cs], in0=gtmp[:, :], in1=sz[:, k, cs], op=ALU.mult), ('gtmp', 'sz'), ('hgt',))
                A(lambda e: e.activation(out=wsc[:, :], in_=gcol[:, :], func=AF.Exp, bias=nalast[:, 0:1], scale=1.0), ('gcol', 'nalast'), ('wsc',))
                V(lambda e: e.tensor_scalar(out=wsc[:, :], in0=wsc[:, :], scalar1=SC, scalar2=None, op0=ALU.mult), ('wsc',), ('wsc',))
                V(lambda e: e.tensor_tensor(out=cw[:, :], in0=m_bc[:, :], in1=nalast[:, :], op=ALU.add), ('m_bc', 'nalast'), ('cw',))
                A(lambda e: e.activation(out=cw[:, :], in_=cw[:, :], func=AF.Exp), ('cw',), ('cw',))
                V(lambda e, n=n: e.tensor_scalar(out=kw[:, :], in0=k_tm[:, n, :], scalar1=wsc[:, 0:1], scalar2=None, op0=ALU.mult), (('k_tm', n), 'wsc'), ('kw',))
                for dk in range(KC):
                    for eh in range(NEH):
                        es = slice(eh * EW, (eh + 1) * EW)
                        bank, bkey = c.psf.next()
                        mm_group(bank[:, 0:EW], [(kw[:, dk * 128:(dk + 1) * 128], v_tm[:, n, es])], ('kw', ('v_tm', n)), (bkey,))
                        V(lambda e, bank=bank, dk=dk, es=es: e.scalar_tensor_tensor(out=Cf[:, dk, es], in0=Cf[:, dk, es], scalar=cw[:, 0:1], in1=bank[:, 0:EW],
                                                                                    op0=ALU.mult, op1=ALU.add), (bkey, 'cw', ('Cf', dk)), (('Cf', dk),))
                    A(lambda e, dk=dk: e.activation(out=Cb[:, dk, :], in_=Cf[:, dk, :], func=AF.Copy), (('Cf', dk),), ('Cb',))
                bank, bkey = c.psf.next()
                for dk in range(KC):
                    mm_group(bank[:, dk:dk + 1], [(kw[:, dk * 128:(dk + 1) * 128], ones_b[:, 0:1])], ('kw', 'ones_b'), (bkey,))
                V(lambda e, bank=bank: e.scalar_tensor_tensor(out=nf[:, :], in0=nf[:, :], scalar=cw[:, 0:1], in1=bank[:, 0:KC], op0=ALU.mult, op1=ALU.add),
                  (bkey, 'cw', 'nf'), ('nf',))
                A(lambda e: e.activation(out=nb[:, :], in_=nf[:, :], func=AF.Copy), ('nf',), ('nb',))
                V(lambda e: e.tensor_copy(out=m_bc[:, :], in_=mnew[:, :]), ('mnew', 'cw', 'iw', 'acol'), ('m_bc',))
            dma('sp', hg_d[:, t0:t0 + T].rearrange("(k p) t -> p k t", p=128), hgt[:], ('hgt',), (('hgo', ti),))
        if mode == 1:
            P.add('sp', None, [('pg', ti) for ti in range(NTILES)], ())
        else:
            P.add('sp', None, [('hgo', ti) for ti in range(NTILES)], ())
        P.build(nc, stack)
    return nc, P


def blockdiag(w, h, dh):
    KC = dh // 128
    out = np.zeros((128, KC, 128), np.float32)
    for k in range(KC):
        base = (h * dh + k * 128) // 4
        for b in range(32):
            out[b * 4:(b + 1) * 4, k, b * 4:(b + 1) * 4] = w[base + b]
    return out


def l12_inputs(cfg, inp, h, mode, pg_all=None):
    D, S = cfg['D'], cfg['S']
    INNER = 2 * D
    dh = INNER // 8
    KC = dh // 128
    hs = slice(h * dh, (h + 1) * dh)
    m = {}
    m["xT"] = cfg['_xT']
    w_in = inp["l0_mix_w_in"]
    m["w_in_h"] = np.ascontiguousarray(np.concatenate([w_in[:, hs], w_in[:, INNER + h * dh:INNER + (h + 1) * dh]], axis=1))
    col = lambda v: np.ascontiguousarray(v.reshape(-1, 128).T)
    m["convw"] = np.ascontiguousarray(np.transpose(inp["l0_conv_w"][:, hs].reshape(4, KC, 128), (2, 1, 0)))
    m["convb"] = col(inp["l0_conv_b"][hs])
    m["wq_bd"] = blockdiag(inp["l0_w_q"], h, dh); m["wk_bd"] = blockdiag(inp["l0_w_k"], h, dh); m["wv_bd"] = blockdiag(inp["l0_w_v"], h, dh)
    m["identb"] = np.eye(128, dtype=np.float32).astype(NPBF)
    m["identf"] = np.eye(128, dtype=np.float32)
    if mode == 1:
        wg = inp["l0_w_gates"][:, hs, :]
        m["wg"] = np.ascontiguousarray(np.transpose(wg.reshape(3, KC, 128, 16), (2, 0, 1, 3)))
    else:
        sel = np.stack([pg_all[:, h, :], pg_all[:, 8 + h, :]], axis=-1)
        m["pgs"] = np.ascontiguousarray(np.transpose(sel, (1, 0, 2)).reshape(S, 16))
        gb = np.empty((128, 2), np.float32)
        gb[:, 0] = inp["l0_b_igate"][h]; gb[:, 1] = inp["l0_b_fgate"][h]
        m["gbias"] = gb
        m["gn"] = inp["l0_head_norm_g"][hs].reshape(1, -1)
        m["skipcol"] = col(inp["l0_skip"][hs])
        m["triu"] = np.triu(np.ones((128, 128), np.float32))
        m["negmask_ts"] = np.where(np.tril(np.ones((128, 128), bool)), 0.0, -1e30).astype(np.float32)
        m["negmask_st"] = np.where(np.triu(np.ones((128, 128), bool)), 0.0, -1e30).astype(np.float32)
    return m


def build_l3(cfg):
    D, TOK, T, FF, E, EFF = cfg['D'], cfg['TOK'], cfg['T'], cfg['FF'], cfg['E'], cfg['EFF']
    KH = cfg['KH']
    STOP = cfg.get('STOP', 99)
    INNER = 2 * D
    SGW = 2 * D
    G = 8
    GD = SGW // G
    KD = D // 128
    NT = T // 128
    NTILES = TOK // T
    FB = KH * 128
    NKP = KD // KH
    NDB = D // 512
    NSB = D // 512
    NVB = SGW // 512
    assert D % 512 == 0 and FF % FB == 0 and EFF % FB == 0 and INNER % FB == 0 and KD % KH == 0 and FB % 512 == 0
    assert GD % 128 == 0
    nc = bass.Bass("TRN2", target_bir_lowering=False)
    dr = lambda name, shape, dt=F32: nc.dram_tensor(name, shape, dt, kind="ExternalInput").ap()
    x_d = dr("x", [TOK, D])
    hg_d = dr("hgT", [INNER, TOK], BF16)
    wout0 = dr("l0_mix_w_out", [INNER, D])
    w1_d = dr("l0_ffn_w1", [D, FF]); w3_d = dr("l0_ffn_w3", [D, FF]); w2_d = dr("l0_ffn_w2", [FF, D])
    win1 = dr("l1_mix_w_in", [D, 2 * SGW]); bin1 = dr("l1_mix_b_in", [1, 2 * SGW])
    sgw_d = dr("l1_sg_wT", [128, G, 128])
    sgb_d = dr("l1_sg_b", [1, G * 128])
    wout1 = dr("l1_mix_w_out", [SGW, D])
    rw_d = dr("l1_router_w", [D, E]); rb_d = dr("l1_router_b", [1, E])
    ew1 = dr("l1_exp_w1", [E * D, EFF]); ew3 = dr("l1_exp_w3", [E * D, EFF]); ew2 = dr("l1_exp_w2", [E * EFF, D])
    lnp = {}
    for nm in ("l0_ln1", "l0_ln2", "l1_ln1", "l1_ln2"):
        lnp[nm] = (dr(nm + "_g", [1, D]), dr(nm + "_b", [1, D]))
    ucol_d = dr("ucol_h", [128, SGW // 128]); ngcol_d = dr("ngcol_h", [128, SGW // 128]); nbcol_d = dr("nbcol_h", [128, SGW // 128])
    identb_d = dr("identb", [128, 128], BF16); identf_d = dr("identf", [128, 128]); causal_d = dr("causalT", [128, 128])
    y_d = nc.dram_tensor("y", [TOK, D], F32, kind="ExternalOutput").ap()

    P = Prog()
    with ExitStack() as stack:
        c = mk_common(nc, stack, P)
        xres = sb(c, "xres", [128, NT, D], F32)
        xT = sb(c, "xT", [128, KD, T], BF16)
        xb = sb(c, "xb", [128, D], BF16)
        wring = Ring([sb(c, "wp%d" % i, [128, KH, 512], BF16) for i in range(cfg.get('NW', 2))], "wp")
        hring = Ring([sb(c, "hT%d" % i, [128, KH, T], BF16) for i in range(2)], "hT")
        gbc = sb(c, "gbc", [128, D], F32)
        s1 = sb(c, "s1", [128, 4, T], F32)
        identb = sb(c, "identb_s", [128, 128], BF16); identf = sb(c, "identf_s", [128, 128], F32)
        causal = sb(c, "causal_s", [128, 128], F32)
        ones_f = sb(c, "ones_f", [128, 128], F32)
        epsc = sb(c, "epsc", [128, 1], F32)
        stats = sb(c, "stats", [128, NSB, 6], F32)
        vstats = sb(c, "vstats", [128, NT, NVB, 6], F32)
        mv = sb(c, "mv", [128, 2], F32); rstd = sb(c, "rstd", [128, 1], F32)
        vst = sb(c, "vst", [128, NT, SGW], BF16)
        vtmp = Ring([sb(c, "vtmp%d" % i, [128, 512], F32) for i in range(2)], "vtmp")
        vt2 = sb(c, "vt2", [128, 512], F32)
        bias_r = Ring([sb(c, "biasr%d" % i, [1, 512], F32) for i in range(2)], "biasr")
        sgwT = sb(c, "sgwT", [128, G, 128], F32); sgwTb = sb(c, "sgwTb", [128, G, 128], BF16)
        rbc = sb(c, "rbc", [128, G * 128], F32)
        bsbc = sb(c, "bsbc", [128, G * 128], F32)
        sgb_row = sb(c, "sgb_row", [1, G * 128], F32)
        ucol = sb(c, "ucol", [128, SGW // 128], F32)
        ngcol = sb(c, "ngcol", [128, SGW // 128], F32); nbcol = sb(c, "nbcol", [128, SGW // 128], F32)
        uTr = Ring([sb(c, "uT%d" % i, [128, T], F32) for i in range(2)], "uT")
        ut2 = sb(c, "ut2", [128, T], F32)
        t1 = sb(c, "t1", [128, 128], F32); t2 = sb(c, "t2", [128, 128], F32)
        xT32 = sb(c, "xT32", [128, min(KD, 16), 128], F32)
        rw32 = sb(c, "rw32", [128, KD, E], F32)
        rb_bc = sb(c, "rb_bc", [128, E], F32)
        lg = sb(c, "lg", [128, E], F32); lg2 = sb(c, "lg2", [128, E], F32); msk = sb(c, "msk", [128, E], F32)
        m1 = sb(c, "m1", [128, 1], F32); m2 = sb(c, "m2", [128, 1], F32); ssum = sb(c, "ssum", [128, 1], F32)
        gates = sb(c, "gates", [128, NT, E], F32)

        def dma(q, out, in_, reads, writes):
            P.add(q, lambda e, o=out, i=in_: e.dma_start(out=o, in_=i), reads=reads, writes=writes, dma=True)

        def load_wpiece(W, r0, c0):
            t, key = wring.next()
            src = W[r0:r0 + KH * 128, c0:c0 + 512].rearrange("(k p) c -> p k c", p=128)
            dma('pool', t[:], src, reads=(), writes=(key,))
            return t, key

        def mm_group(outap, pairs, reads, writes, start=True, stop=True):
            def fn(e, outap=outap, pairs=pairs, start=start, stop=stop):
                ins = None
                for idx, (l, r) in enumerate(pairs):
                    ins = e.matmul(outap, l, r, start=(start and idx == 0), stop=(stop and idx == len(pairs) - 1))
                return ins
            P.add('pe', fn, reads=reads, writes=writes)

        def k1(W, r_base, c0, src, srckeys, evac):
            banks = [c.psf.next() for _ in range(4)]
            for kp in range(NKP):
                wt, wkey = load_wpiece(W, r_base + kp * FB, c0)
                for ci in range(4):
                    pairs = [(wt[:, k, ci * 128:(ci + 1) * 128], src[:, kp * KH + k, 0:T]) for k in range(KH)]
                    mm_group(banks[ci][0][:, 0:T], pairs, reads=(wkey,) + tuple(srckeys), writes=(banks[ci][1],),
                             start=(kp == 0), stop=(kp == NKP - 1))
            for ci in range(4):
                evac(ci, banks[ci][0][:, 0:T], banks[ci][1])

        def k2(W, r0, hsrc, hkey, gate_e=None):
            for db in range(NDB):
                wt, wkey = load_wpiece(W, r0, db * 512)
                for n in range(NT):
                    bank, bkey = c.psf.next()
                    pairs = [(hsrc[:, k, n * 128:(n + 1) * 128], wt[:, k, :]) for k in range(KH)]
                    mm_group(bank[:, :], pairs, reads=(wkey, hkey), writes=(bkey,))
                    xs = xres[:, n, db * 512:(db + 1) * 512]
                    xk = ('xres', n, db)
                    if gate_e is None:
                        P.add('dve', lambda e, b=bank, xs=xs: e.tensor_tensor(out=xs, in0=b[:, :], in1=xs, op=ALU.add),
                              reads=(bkey, xk), writes=(xk,))
                    else:
                        gs = gates[:, n, gate_e:gate_e + 1]
                        P.add('dve', lambda e, b=bank, xs=xs, gs=gs: e.scalar_tensor_tensor(
                            out=xs, in0=b[:, :], scalar=gs, in1=xs, op0=ALU.mult, op1=ALU.add),
                            reads=(bkey, xk, 'gates'), writes=(xk,))

        XT_KEYS = [('xT', n) for n in range(NT)]

        def transposes_to_xT(n):
            for k0 in range(0, KD, 8):
                kk = min(8, KD - k0)
                pb, pkey = c.psb.next()

                def tfn(e, pb=pb, k0=k0, kk=kk):
                    ins = None
                    for k in range(kk):
                        ins = e.transpose(out=pb[:, k * 128:(k + 1) * 128], in_=xb[:, (k0 + k) * 128:(k0 + k + 1) * 128], identity=identb[:, :])
                    return ins
                P.add('pe', tfn, reads=('xb', 'identb'), writes=(pkey,))
                P.add('act', lambda e, pb=pb, k0=k0, kk=kk, n=n: e.activation(
                    out=xT[:, k0:k0 + kk, n * 128:(n + 1) * 128], in_=pb[:, 0:kk * 128].rearrange("p (k t) -> p k t", t=128), func=AF.Copy),
                    reads=(pkey,), writes=(('xT', n),))

        def layer_norm(g_d, b_d, final=False, tile_i=0, router=False):
            dma('sp', gbc[:], bcast_rows(g_d, 128), (), ('gbc',))
            for n in range(NT):
                xk = [('xres', n, db) for db in range(NDB)]
                xn = xres[:, n, :]
                for j in range(NSB):
                    P.add('dve', lambda e, j=j, xn=xn: e.bn_stats(out=stats[:, j, :], in_=xn[:, j * 512:(j + 1) * 512]),
                          reads=(('xres', n, j),), writes=(('stats', j),))
                P.add('dve', lambda e: e.bn_aggr(out=mv[:, :], in_=stats[:, :, :].rearrange("p a b -> p (a b)")),
                      reads=[('stats', j) for j in range(NSB)], writes=('mv',))
                P.add('act', lambda e: e.activation(out=rstd[:, :], in_=mv[:, 1:2], func=AF.Sqrt, bias=epsc[:, 0:1], scale=1.0),
                      reads=('mv', 'epsc'), writes=('rstd',))
                P.add('dve', lambda e: e.reciprocal(out=rstd[:, :], in_=rstd[:, :]), reads=('rstd',), writes=('rstd',))
                P.add('dve', lambda e, xn=xn: e.tensor_scalar(out=xn, in0=xn, scalar1=mv[:, 0:1], scalar2=rstd[:, 0:1], op0=ALU.subtract, op1=ALU.mult),
                      reads=['mv', 'rstd'] + xk, writes=xk)
                P.add('dve', lambda e, xn=xn: e.tensor_tensor(out=xn, in0=xn, in1=gbc[:, :], op=ALU.mult), reads=['gbc'] + xk, writes=xk)
            dma('sp', gbc[:], bcast_rows(b_d, 128), (), ('gbc',))
            for n in range(NT):
                xk = [('xres', n, db) for db in range(NDB)]
                xn = xres[:, n, :]
                P.add('dve', lambda e, xn=xn: e.tensor_tensor(out=xn, in0=xn, in1=gbc[:, :], op=ALU.add), reads=['gbc'] + xk, writes=xk)
                if final:
                    r0 = tile_i * T + n * 128
                    dma('sp', y_d[r0:r0 + 128, :], xn, reads=xk, writes=(('y', tile_i, n),))
                    continue
                P.add('act', lambda e, xn=xn: e.activation(out=xb[:, :], in_=xn, func=AF.Copy), reads=xk, writes=('xb',))
                transposes_to_xT(n)
                if router:
                    router_gates(n, xn, xk)
                P.add('act', lambda e, xn=xn: e.mul(out=xn, in_=xn, mul=ALPHA), reads=xk, writes=xk)

        def router_gates(n, xn, xk):
            KDH = min(KD, 16)
            V = lambda fn, r, w: P.add('dve', fn, r, w)
            for hh_ in range(KD // KDH):
                for k0 in range(hh_ * KDH, (hh_ + 1) * KDH, 4):
                    kk = 4
                    bank, bkey = c.psf.next()

                    def tfn(e, bank=bank, k0=k0, kk=kk, xn=xn):
                        ins = None
                        for k in range(kk):
                            ins = e.transpose(out=bank[:, k * 128:(k + 1) * 128], in_=xn[:, (k0 + k) * 128:(k0 + k + 1) * 128], identity=identf[:, :])
                        return ins
                    P.add('pe', tfn, reads=xk + ['identf'], writes=(bkey,))
                    kl = k0 - hh_ * KDH
                    P.add('act', lambda e, bank=bank, kl=kl, kk=kk: e.activation(
                        out=xT32[:, kl:kl + kk, :], in_=bank[:, 0:kk * 128].rearrange("p (k t) -> p k t", t=128), func=AF.Copy),
                        reads=(bkey,), writes=('xT32',))
                lbank, lkey = c.psf.next()
                mm_group(lbank[:, 0:E], [(xT32[:, k, :], rw32[:, hh_ * KDH + k, :]) for k in range(KDH)], ('xT32', 'rw32'), (lkey,))
                if hh_ == 0:
                    V(lambda e, lbank=lbank: e.tensor_tensor(out=lg[:, :], in0=lbank[:, 0:E], in1=rb_bc[:, :], op=ALU.add), (lkey, 'rb_bc'), ('lg',))
                else:
                    V(lambda e, lbank=lbank: e.tensor_tensor(out=lg[:, :], in0=lbank[:, 0:E], in1=lg[:, :], op=ALU.add), (lkey, 'lg'), ('lg',))
            V(lambda e: e.tensor_reduce(out=m1[:, :], in_=lg[:, :], axis=AX.X, op=ALU.max), ('lg',), ('m1',))
            V(lambda e: e.tensor_scalar(out=msk[:, :], in0=lg[:, :], scalar1=m1[:, 0:1], scalar2=-1e30, op0=ALU.is_ge, op1=ALU.mult), ('lg', 'm1'), ('msk',))
            V(lambda e: e.tensor_tensor(out=lg2[:, :], in0=lg[:, :], in1=msk[:, :], op=ALU.add), ('lg', 'msk'), ('lg2',))
            V(lambda e: e.tensor_reduce(out=m2[:, :], in_=lg2[:, :], axis=AX.X, op=ALU.max), ('lg2',), ('m2',))
            V(lambda e: e.tensor_scalar(out=msk[:, :], in0=lg[:, :], scalar1=m2[:, 0:1], scalar2=None, op0=ALU.is_ge), ('lg', 'm2'), ('msk',))
            V(lambda e: e.tensor_scalar(out=lg2[:, :], in0=lg[:, :], scalar1=m1[:, 0:1], scalar2=None, op0=ALU.subtract), ('lg', 'm1'), ('lg2',))
            P.add('act', lambda e: e.activation(out=lg2[:, :], in_=lg2[:, :], func=AF.Exp), ('lg2',), ('lg2',))
            V(lambda e: e.tensor_tensor(out=lg2[:, :], in0=lg2[:, :], in1=msk[:, :], op=ALU.mult), ('lg2', 'msk'), ('lg2',))
            V(lambda e: e.tensor_reduce(out=ssum[:, :], in_=lg2[:, :], axis=AX.X, op=ALU.add), ('lg2',), ('ssum',))
            V(lambda e: e.reciprocal(out=ssum[:, :], in_=ssum[:, :]), ('ssum',), ('ssum',))
            V(lambda e, n=n: e.tensor_scalar(out=gates[:, n, :], in0=lg2[:, :], scalar1=ssum[:, 0:1], scalar2=None, op0=ALU.mult), ('lg2', 'ssum'), ('gates',))

        def gelu_tanh(out_ap, outkey, src_ap, srckey, bias_ap, tmpa, tmpakey, tmpb, tmpbkey, extra_reads=()):
            if bias_ap is not None:
                P.add('act', lambda e: e.activation(out=tmpa, in_=src_ap, func=AF.Identity, bias=bias_ap, scale=1.0),
                      reads=(srckey,) + tuple(extra_reads), writes=(tmpakey,))
            else:
                P.add('act', lambda e: e.activation(out=tmpa, in_=src_ap, func=AF.Copy), reads=(srckey,), writes=(tmpakey,))
            P.add('dve', lambda e: e.tensor_tensor(out=tmpb, in0=tmpa, in1=tmpa, op=ALU.mult), reads=(tmpakey,), writes=(tmpbkey,))
            P.add('dve', lambda e: e.tensor_scalar(out=tmpb, in0=tmpb, scalar1=0.044715, scalar2=1.0, op0=ALU.mult, op1=ALU.add),
                  reads=(tmpbkey,), writes=(tmpbkey,))
            P.add('dve', lambda e: e.tensor_tensor(out=tmpb, in0=tmpb, in1=tmpa, op=ALU.mult), reads=(tmpbkey, tmpakey), writes=(tmpbkey,))
            P.add('act', lambda e: e.activation(out=tmpb, in_=tmpb, func=AF.Sigmoid, scale=1.5957691216057308), reads=(tmpbkey,), writes=(tmpbkey,))
            P.add('dve', lambda e: e.tensor_tensor(out=out_ap, in0=tmpb, in1=tmpa, op=ALU.mult), reads=(tmpbkey, tmpakey), writes=(outkey,))

        def ffn(W1, W3, W2, r1_base, r2_base, FFdim, gate_e=None):
            for fb in range(FFdim // FB):
                ht, hkey = hring.next()
                for cg in range(FB // 512):
                    c0 = fb * FB + cg * 512

                    def ev1(ci, bank, bkey):
                        P.add('act', lambda e, ci=ci, bank=bank: e.activation(out=s1[:, ci, :], in_=bank, func=AF.Silu),
                              reads=(bkey,), writes=(('s1', ci),))
                    k1(W1, r1_base, c0, xT, XT_KEYS, ev1)

                    def ev3(ci, bank, bkey, cg=cg, ht=ht, hkey=hkey):
                        P.add('dve', lambda e, ci=ci, bank=bank: e.tensor_tensor(out=ht[:, cg * 4 + ci, :], in0=bank, in1=s1[:, ci, :], op=ALU.mult),
                              reads=(bkey, ('s1', ci)), writes=(hkey,))
                    k1(W3, r1_base, c0, xT, XT_KEYS, ev3)
                k2(W2, r2_base + fb * FB, ht, hkey, gate_e=gate_e)

        dma('sp', identb[:], identb_d, (), ('identb',))
        dma('sp', identf[:], identf_d, (), ('identf',))
        dma('sp', causal[:], causal_d, (), ('causal',))
        P.add('dve', lambda e: e.memset(ones_f[:, :], 1.0), (), ('ones_f',))
        P.add('dve', lambda e: e.memset(epsc[:, :], LN_EPS), (), ('epsc',))
        dma('sp', sgwT[:], sgw_d, (), ('sgwT',))
        for g in range(G):
            P.add('dve', lambda e, g=g: e.tensor_tensor(out=sgwT[:, g, :], in0=sgwT[:, g, :], in1=causal[:, :], op=ALU.mult),
                  ('sgwT', 'causal'), ('sgwT',))
        P.add('act', lambda e: e.activation(out=sgwTb[:], in_=sgwT[:], func=AF.Copy), ('sgwT',), ('sgwTb',))
        for g0 in range(0, G, 4):
            bank, bkey = c.psf.next()
            mm_group(bank[:, :], [(ones_f[:, :], sgwT[:, g0:g0 + 4, :].rearrange("p g t -> p (g t)"))], ('ones_f', 'sgwT'), (bkey,))
            P.add('act', lambda e, bank=bank, g0=g0: e.activation(out=rbc[:, g0 * 128:(g0 + 4) * 128], in_=bank[:, :], func=AF.Copy),
                  (bkey,), ('rbc',))
        dma('sp', sgb_row[:], sgb_d, (), ('sgb_row',))
        for j0 in range(0, G * 128, 512):
            bank, bkey = c.psf.next()
            mm_group(bank[:, :], [(ones_f[0:1, :], sgb_row[0:1, j0:j0 + 512])], ('ones_f', 'sgb_row'), (bkey,))
            P.add('act', lambda e, bank=bank, j0=j0: e.activation(out=bsbc[:, j0:j0 + 512], in_=bank[:, :], func=AF.Copy), (bkey,), ('bsbc',))
        dma('sp', ucol[:], ucol_d, (), ('ucol',))
        dma('sp', ngcol[:], ngcol_d, (), ('ngcol',))
        dma('sp', nbcol[:], nbcol_d, (), ('nbcol',))
        dma('sp', rw32[:], rw_d.rearrange("(k p) e -> p k e", p=128), (), ('rw32',))
        dma('sp', rb_bc[:], bcast_rows(rb_d, 128), (), ('rb_bc',))

        def finish_tile(ti):
            for n in range(NT):
                xk = [('xres', n, db) for db in range(NDB)]
                r0 = ti * T + n * 128
                dma('sp', y_d[r0:r0 + 128, :], xres[:, n, :], reads=xk, writes=(('y', ti, n),))

        for ti in range(NTILES):
            t0 = ti * T
            for n in range(NT):
                xk = [('xres', n, db) for db in range(NDB)]
                dma('sp', xres[:, n, :], x_d[t0 + n * 128:t0 + (n + 1) * 128, :], (), xk)
                P.add('act', lambda e, n=n: e.mul(out=xres[:, n, :], in_=xres[:, n, :], mul=ALPHA), xk, xk)
            for rb in range(INNER // FB):
                ht, hkey = hring.next()
                dma('sp', ht[:], hg_d[rb * FB:(rb + 1) * FB, t0:t0 + T].rearrange("(k p) t -> p k t", p=128), (), (hkey,))
                k2(wout0, rb * FB, ht, hkey)
            if STOP == 1:
                layer_norm(*lnp["l0_ln1"], final=True, tile_i=ti); continue
            layer_norm(*lnp["l0_ln1"])
            ffn(w1_d, w3_d, w2_d, 0, 0, FF)
            if STOP == 2:
                layer_norm(*lnp["l0_ln2"], final=True, tile_i=ti); continue
            layer_norm(*lnp["l0_ln2"])

            for fg in range(NVB):
                c0 = SGW + fg * 512
                bt, bkey_b = bias_r.next()
                dma('sp', bt[:], bin1[0:1, c0:c0 + 512], (), (bkey_b,))
                banks = [c.psf.next() for _ in range(NT)]
                for n in range(NT):
                    mm_group(banks[n][0][:, :], [(ones_f[0:1, :], bt[0:1, :])], ('ones_f', bkey_b), (banks[n][1],), start=True, stop=False)
                for kp in range(NKP):
                    wt, wkey = load_wpiece(win1, kp * FB, c0)
                    for n in range(NT):
                        pairs = [(xT[:, kp * KH + k, n * 128:(n + 1) * 128], wt[:, k, :]) for k in range(KH)]
                        mm_group(banks[n][0][:, :], pairs, (wkey, ('xT', n)), (banks[n][1],), start=False, stop=(kp == NKP - 1))
                for n in range(NT):
                    vt, vkey = vtmp.next()
                    gelu_tanh(vt[:, :], vkey, banks[n][0][:, :], banks[n][1], None, vt[:, :], vkey, vt2[:, :], 'vt2')
                    P.add('dve', lambda e, vt=vt, n=n, fg=fg: e.bn_stats(out=vstats[:, n, fg, :], in_=vt[:, :]), (vkey,), (('vstats', n),))
                    P.add('act', lambda e, vt=vt, n=n, fg=fg: e.activation(out=vst[:, n, fg * 512:(fg + 1) * 512], in_=vt[:, :], func=AF.Copy),
                          (vkey,), (('vst', n),))
            for n in range(NT):
                P.add('dve', lambda e, n=n: e.bn_aggr(out=mv[:, :], in_=vstats[:, n, :, :].rearrange("p a b -> p (a b)")), (('vstats', n),), ('mv',))
                P.add('act', lambda e: e.activation(out=rstd[:, :], in_=mv[:, 1:2], func=AF.Sqrt, bias=epsc[:, 0:1], scale=1.0),
                      ('mv', 'epsc'), ('rstd',))
                P.add('dve', lambda e: e.reciprocal(out=rstd[:, :], in_=rstd[:, :]), ('rstd',), ('rstd',))
                P.add('dve', lambda e, n=n: e.tensor_scalar(out=vst[:, n, :], in0=vst[:, n, :], scalar1=mv[:, 0:1], scalar2=rstd[:, 0:1],
                                                             op0=ALU.subtract, op1=ALU.mult), ('mv', 'rstd', ('vst', n)), (('vst', n),))
            VST_KEYS = [('vst', n) for n in range(NT)]
            for rb in range(SGW // FB):
                gt, gkey = hring.next()
                for cg in range(FB // 512):
                    c0 = rb * FB + cg * 512

                    def evu(ci, bank, bkey, c0=c0, cg=cg, gt=gt, gkey=gkey):
                        fc = c0 // 128 + ci
                        g = (fc * 128) // GD
                        ut, ukey = uTr.next()
                        gelu_tanh(ut[:, :], ukey, bank, bkey, ucol[:, fc:fc + 1], ut[:, :], ukey, ut2[:, :], 'ut2', extra_reads=('ucol',))
                        P.add('dve', lambda e, fc=fc, g=g: e.scalar_tensor_tensor(
                            out=t1[:, :], in0=rbc[:, g * 128:(g + 1) * 128], scalar=nbcol[:, fc:fc + 1], in1=bsbc[:, g * 128:(g + 1) * 128],
                            op0=ALU.mult, op1=ALU.add), ('rbc', 'nbcol', 'bsbc'), ('t1',))
                        pbank, pkey = c.psf.next()
                        for n in range(NT):
                            mm_group(pbank[:, n * 128:(n + 1) * 128], [(vst[:, n, fc * 128:(fc + 1) * 128], sgwTb[:, g, :])],
                                     (('vst', n), 'sgwTb'), (pkey,))
                        for n in range(NT):
                            P.add('dve', lambda e, n=n, fc=fc, pbank=pbank: e.scalar_tensor_tensor(
                                out=t2[:, :], in0=pbank[:, n * 128:(n + 1) * 128], scalar=ngcol[:, fc:fc + 1], in1=t1[:, :],
                                op0=ALU.mult, op1=ALU.add), (pkey, 'ngcol', 't1'), ('t2',))
                            P.add('dve', lambda e, n=n, ut=ut, ci=ci: e.tensor_tensor(
                                out=gt[:, cg * 4 + ci, n * 128:(n + 1) * 128], in0=t2[:, :], in1=ut[:, n * 128:(n + 1) * 128], op=ALU.mult),
                                ('t2', ukey), (gkey,))
                    k1(win1, 0, c0, xT, XT_KEYS, evu)
                k2(wout1, rb * FB, gt, gkey)
            if STOP == 3:
                layer_norm(*lnp["l1_ln1"], final=True, tile_i=ti); continue
            layer_norm(*lnp["l1_ln1"], router=True)
            for ex in range(E):
                ffn(ew1, ew3, ew2, ex * D, ex * EFF, EFF, gate_e=ex)
            layer_norm(*lnp["l1_ln2"], final=True, tile_i=ti)
        P.add('sp', None, [('y', ti, n) for ti in range(NTILES) for n in range(NT)], ())
        P.build(nc, stack)
    return nc, P


def l3_inputs(cfg, inp, hgT_full, core):
    D, TOK = cfg['D'], cfg['TOK']
    E = cfg['E']
    sl = slice(core * TOK, (core + 1) * TOK)
    m = {}
    m["x"] = np.ascontiguousarray(inp["x"].reshape(-1, D)[sl])
    m["hgT"] = np.ascontiguousarray(hgT_full[:, sl])
    for k in ("l0_mix_w_out", "l0_ffn_w1", "l0_ffn_w3", "l0_ffn_w2", "l1_mix_w_in", "l1_mix_w_out", "l1_router_w"):
        m[k] = inp[k]
    m["l1_mix_b_in"] = inp["l1_mix_b_in"].reshape(1, -1)
    m["l1_sg_wT"] = np.ascontiguousarray(np.transpose(inp["l1_sg_w"], (2, 0, 1)))
    m["l1_sg_b"] = inp["l1_sg_b"].reshape(1, -1)
    m["l1_router_b"] = inp["l1_router_b"].reshape(1, -1)
    m["l1_exp_w1"] = inp["l1_exp_w1"].reshape(E * D, -1)
    m["l1_exp_w3"] = inp["l1_exp_w3"].reshape(E * D, -1)
    m["l1_exp_w2"] = inp["l1_exp_w2"].reshape(-1, D)
    for nm in ("l0_ln1", "l0_ln2", "l1_ln1", "l1_ln2"):
        m[nm + "_g"] = inp[nm + "_g"].reshape(1, -1)
        m[nm + "_b"] = inp[nm + "_b"].reshape(1, -1)
    SGW = 2 * D
    col = lambda v: np.ascontiguousarray(v.reshape(-1, 128).T)
    m["ucol_h"] = col(inp["l1_mix_b_in"][:SGW])
    m["ngcol_h"] = col(inp["l1_sg_norm_g"])
    m["nbcol_h"] = col(inp["l1_sg_norm_b"])
    m["identb"] = np.eye(128, dtype=np.float32).astype(NPBF)
    m["identf"] = np.eye(128, dtype=np.float32)
    m["causalT"] = np.triu(np.ones((128, 128), np.float32))
    return m


def kernel(**inputs):
    inp = {k: np.asarray(v) for k, v in inputs.items()}
    D = 4096
    S = 8192
    cfg12 = dict(D=D, S=S, T=256, KH=16, NW=2)
    cfg12['_xT'] = np.ascontiguousarray(inp['x'].reshape(S, D).T)
    nc1, _ = build_l12(cfg12, 1)
    res = run_bass_kernel_spmd(nc1, [l12_inputs(cfg12, inp, h, 1) for h in range(8)], core_ids=list(range(8)))
    pg_all = np.stack([np.asarray(r["pg"]) for r in res.results], 0)
    del res, nc1
    nc2, _ = build_l12(cfg12, 2)
    res = run_bass_kernel_spmd(nc2, [l12_inputs(cfg12, inp, h, 2, pg_all) for h in range(8)], core_ids=list(range(8)))
    hgT = np.concatenate([np.asarray(r["hgT"]) for r in res.results], 0)
    del res, nc2
    cfg3 = dict(D=D, TOK=1024, T=256, FF=14336, E=8, EFF=4096, KH=16, NW=2)
    nc3, _ = build_l3(cfg3)
    res = run_bass_kernel_spmd(nc3, [l3_inputs(cfg3, inp, hgT, c) for c in range(8)], core_ids=list(range(8)))
    y = np.concatenate([np.asarray(r["y"]) for r in res.results], 0).reshape(1, S, D).astype(np.float32)
    return y
```
